# Optimizing a Trainium2 kernel written in Bass

```python
import math
import jax, jax.numpy as jnp
from jax import lax
import numpy as np

D_MODEL = 2048
BATCH = 1
SEQ = 8192
DEPTH = 2

N_A_LAYERS = DEPTH // 2
N_B_LAYERS = DEPTH - N_A_LAYERS
CONV_WIDTH = 3
HEAD_DIM = 128
N_HEAD_SLOTS = D_MODEL // HEAD_DIM
DILATED_GROUPS = ((128, 1), (512, 4), (2048, 16))
N_GROUPS = len(DILATED_GROUPS)
KV_HEADS = 4
Q_PER_KV = N_HEAD_SLOTS // KV_HEADS
Q_BLOCK = 128
N_EXPERTS = 32
TOP_K = 4
D_EXPERT = D_MODEL
SWIGLU_LIMIT = 7.0
SWIGLU_ALPHA = 1.702
EXPERT_BLOCK = 256
LN_EPS = 1e-5
DEEPNORM_ALPHA = (2.0 * DEPTH) ** 0.25
DEEPNORM_BETA = (8.0 * DEPTH) ** -0.25

kernel_name = "yoco_shortconv_dilated_alibi_moe_deepnorm"


def layer_norm(x, g, b):
    xf = x.astype(jnp.float32)
    mu = jnp.mean(xf, axis=-1, keepdims=True)
    var = jnp.mean(jnp.square(xf - mu), axis=-1, keepdims=True)
    y = (xf - mu) * lax.rsqrt(var + LN_EPS) * g.astype(jnp.float32) + b.astype(jnp.float32)
    return y.astype(x.dtype)


def alibi_slopes(n_heads):
    i = jnp.arange(1, n_heads + 1, dtype=jnp.float32)
    return jnp.exp2(-8.0 * i / n_heads)


def short_conv_mixer(h, w_in, conv_w, w_out):
    b_gate, c_gate, u = jnp.split(h @ w_in, 3, axis=-1)
    v = c_gate * u
    conv = lax.conv_general_dilated(
        v, conv_w[:, None, :].astype(v.dtype), window_strides=(1,),
        padding=((CONV_WIDTH - 1, 0),),
        dimension_numbers=("NWC", "WIO", "NWC"),
        feature_group_count=D_MODEL)
    return (b_gate * conv) @ w_out


def shared_kv(h, w_kv):
    bn, s, _ = h.shape
    kv = (h @ w_kv).reshape(bn, s, 2, N_GROUPS, KV_HEADS, HEAD_DIM)
    return kv[:, :, 0], kv[:, :, 1]


def dilated_attention(h, w_q, w_o, k, v):
    bn, s, _ = h.shape
    q = (h @ w_q).reshape(bn, s, N_GROUPS, KV_HEADS, Q_PER_KV, HEAD_DIM)
    slopes = alibi_slopes(N_HEAD_SLOTS).reshape(KV_HEADS, Q_PER_KV, 1, 1)
    scale = HEAD_DIM ** -0.5
    k_groups = [k[:, :, g] for g in range(N_GROUPS)]
    v_groups = [v[:, :, g] for g in range(N_GROUPS)]

    def block(b):
        t = b * Q_BLOCK + jnp.arange(Q_BLOCK, dtype=jnp.int32)
        q_blk = lax.dynamic_slice_in_dim(q, b * Q_BLOCK, Q_BLOCK, axis=1)
        outs, lses = [], []
        for g, (window, dil) in enumerate(DILATED_GROUPS):
            n_keys = window // dil + 1
            dist = dil * jnp.arange(n_keys, dtype=jnp.int32)
            idx = t[:, None] - dist[None, :]
            valid = idx >= 0
            idx = jnp.maximum(idx, 0)
            kg = jnp.take(k_groups[g], idx, axis=1)
            vg = jnp.take(v_groups[g], idx, axis=1)
            sc = jnp.einsum("bqhrd,bqjhd->bhrqj", q_blk[:, :, g], kg).astype(jnp.float32) * scale
            sc = sc - slopes * dist.astype(jnp.float32)
            sc = jnp.where(valid, sc, -jnp.inf)
            m = jnp.max(sc, axis=-1, keepdims=True)
            p = jnp.exp(sc - m)
            l = jnp.sum(p, axis=-1, keepdims=True)
            o = jnp.einsum("bhrqj,bqjhd->bhrqd", p, vg.astype(jnp.float32)) / l
            outs.append(o)
            lses.append(m + jnp.log(l))
        w = jax.nn.softmax(jnp.stack(lses), axis=0)
        o = jnp.sum(w * jnp.stack(outs), axis=0)
        return o.transpose(0, 3, 1, 2, 4).reshape(bn, Q_BLOCK, N_HEAD_SLOTS * HEAD_DIM)

    n_blocks = s // Q_BLOCK
    o = lax.map(block, jnp.arange(n_blocks, dtype=jnp.int32))
    o = o.transpose(1, 0, 2, 3).reshape(bn, s, N_HEAD_SLOTS * HEAD_DIM).astype(h.dtype)
    return o @ w_o


def moe_ffn(h, router_w, router_b, w_gate, b_gate, w_up, b_up, w_down, b_down):
    bn, s, d = h.shape
    n_tok = bn * s
    xf = h.reshape(n_tok, d)
    logits = (xf @ router_w + router_b).astype(jnp.float32)
    top_val, top_idx = lax.top_k(logits, TOP_K)
    gate_w = jax.nn.softmax(top_val, axis=-1)

    n_assign = n_tok * TOP_K
    e_flat = top_idx.reshape(n_assign).astype(jnp.int32)
    tok_flat = jnp.repeat(jnp.arange(n_tok, dtype=jnp.int32), TOP_K)
    w_flat = gate_w.reshape(n_assign)
    order = jnp.argsort(e_flat)
    e_sorted = e_flat[order]
    counts = jnp.bincount(e_flat, length=N_EXPERTS)
    padded = (counts + EXPERT_BLOCK - 1) // EXPERT_BLOCK * EXPERT_BLOCK
    raw_start = jnp.cumsum(counts) - counts
    pad_end = jnp.cumsum(padded)
    pad_start = pad_end - padded
    rank = jnp.arange(n_assign, dtype=jnp.int32) - raw_start[e_sorted]
    dest = pad_start[e_sorted] + rank
    n_blocks = -(-(n_assign + N_EXPERTS * (EXPERT_BLOCK - 1)) // EXPERT_BLOCK)
    n_rows = n_blocks * EXPERT_BLOCK
    row_tok = jnp.full((n_rows,), n_tok, jnp.int32).at[dest].set(tok_flat[order])
    row_w = jnp.zeros((n_rows,), jnp.float32).at[dest].set(w_flat[order])
    block_start = jnp.arange(n_blocks, dtype=jnp.int32) * EXPERT_BLOCK
    block_exp = jnp.minimum(jnp.searchsorted(pad_end, block_start, side="right"), N_EXPERTS - 1)
    x_rows = jnp.concatenate([xf, jnp.zeros((1, d), xf.dtype)], axis=0)[row_tok]
    x_rows = x_rows.reshape(n_blocks, EXPERT_BLOCK, d)

    def expert_block(args):
        xb, e = args
        g = xb @ w_gate[e] + b_gate[e]
        u = xb @ w_up[e] + b_up[e]
        g = jnp.minimum(g, SWIGLU_LIMIT)
        u = jnp.clip(u, -SWIGLU_LIMIT, SWIGLU_LIMIT)
        glu = g * jax.nn.sigmoid(SWIGLU_ALPHA * g)
        return ((u + 1.0) * glu) @ w_down[e] + b_down[e]

    y_rows = lax.map(expert_block, (x_rows, block_exp)).reshape(n_rows, d)
    out = jnp.zeros((n_tok + 1, d), jnp.float32).at[row_tok].add(
        y_rows.astype(jnp.float32) * row_w[:, None])[:n_tok]
    return out.reshape(bn, s, d).astype(h.dtype)


def setup_inputs(seed: int = 0) -> dict:
    key = jax.random.key(seed)
    ks = jax.random.split(key, 20)
    f32 = jnp.float32
    d, f, e = D_MODEL, D_EXPERT, N_EXPERTS
    attn_w = N_HEAD_SLOTS * HEAD_DIM
    kv_width = N_GROUPS * KV_HEADS * HEAD_DIM

    def nrm(k, shape, scale):
        return jax.random.normal(k, shape, f32) * scale

    x = nrm(ks[0], (BATCH, SEQ, d), 1.0)
    a_w_in = nrm(ks[1], (N_A_LAYERS, d, 3 * d), d ** -0.5)
    a_conv_w = nrm(ks[2], (N_A_LAYERS, CONV_WIDTH, d), CONV_WIDTH ** -0.5)
    a_w_out = nrm(ks[3], (N_A_LAYERS, d, d), d ** -0.5 * DEEPNORM_BETA)
    b_w_q = nrm(ks[4], (N_B_LAYERS, d, N_GROUPS * attn_w), d ** -0.5)
    b_w_o = nrm(ks[5], (N_B_LAYERS, attn_w, d), attn_w ** -0.5 * DEEPNORM_BETA)
    kv_w = jnp.concatenate([nrm(ks[6], (d, kv_width), d ** -0.5),
                            nrm(ks[7], (d, kv_width), d ** -0.5 * DEEPNORM_BETA)], axis=1)
    ln_mix_g = 1.0 + nrm(ks[8], (DEPTH, d), 0.02)
    ln_mix_b = nrm(ks[9], (DEPTH, d), 0.02)
    ln_ffn_g = 1.0 + nrm(ks[10], (DEPTH, d), 0.02)
    ln_ffn_b = nrm(ks[11], (DEPTH, d), 0.02)
    router_w = nrm(ks[12], (DEPTH, d, e), d ** -0.5)
    router_b = nrm(ks[13], (DEPTH, e), 0.01)
    exp_w_gate = nrm(ks[14], (DEPTH, e, d, f), d ** -0.5)
    exp_b_gate = nrm(ks[15], (DEPTH, e, f), 0.02)
    exp_w_up = nrm(ks[16], (DEPTH, e, d, f), d ** -0.5)
    exp_b_up = nrm(ks[17], (DEPTH, e, f), 0.02)
    exp_w_down = nrm(ks[18], (DEPTH, e, f, d), f ** -0.5 * DEEPNORM_BETA)
    exp_b_down = nrm(ks[19], (DEPTH, e, d), 0.02)
    return {"x": x, "a_w_in": a_w_in, "a_conv_w": a_conv_w, "a_w_out": a_w_out,
            "b_w_q": b_w_q, "b_w_o": b_w_o, "kv_w": kv_w,
            "ln_mix_g": ln_mix_g, "ln_mix_b": ln_mix_b, "ln_ffn_g": ln_ffn_g, "ln_ffn_b": ln_ffn_b,
            "router_w": router_w, "router_b": router_b,
            "exp_w_gate": exp_w_gate, "exp_b_gate": exp_b_gate,
            "exp_w_up": exp_w_up, "exp_b_up": exp_b_up,
            "exp_w_down": exp_w_down, "exp_b_down": exp_b_down}


def reference(x, a_w_in, a_conv_w, a_w_out, b_w_q, b_w_o, kv_w,
              ln_mix_g, ln_mix_b, ln_ffn_g, ln_ffn_b, router_w, router_b,
              exp_w_gate, exp_b_gate, exp_w_up, exp_b_up, exp_w_down, exp_b_down):
    h = x
    k_shared = v_shared = None
    for layer in range(DEPTH):
        if layer < N_A_LAYERS:
            mix = short_conv_mixer(h, a_w_in[layer], a_conv_w[layer], a_w_out[layer])
        else:
            if layer == N_A_LAYERS:
                k_shared, v_shared = shared_kv(h, kv_w)
            j = layer - N_A_LAYERS
            mix = dilated_attention(h, b_w_q[j], b_w_o[j], k_shared, v_shared)
        h = layer_norm(DEEPNORM_ALPHA * h + mix, ln_mix_g[layer], ln_mix_b[layer])
        ffn = moe_ffn(h, router_w[layer], router_b[layer],
                      exp_w_gate[layer], exp_b_gate[layer], exp_w_up[layer], exp_b_up[layer],
                      exp_w_down[layer], exp_b_down[layer])
        h = layer_norm(DEEPNORM_ALPHA * h + ffn, ln_ffn_g[layer], ln_ffn_b[layer])
    return h
```

```python
from contextlib import ExitStack
import numpy as np
import ml_dtypes
import concourse.bass as bass
import concourse.mybir as mybir
from concourse.bass_utils import run_bass_kernel_spmd

F32 = mybir.dt.float32
BF16 = mybir.dt.bfloat16
F16 = mybir.dt.float16
AF = mybir.ActivationFunctionType
ALU = mybir.AluOpType
AX = mybir.AxisListType

NCORE = 8
T = 1024
NT = 8
D = 2048
KD = 16
E = 32
CAP = 256
ALPHA = 4.0 ** 0.25
EPS = 1e-5
SCALE = 128.0 ** -0.5
GROUPS = ((128, 1), (512, 4), (2048, 16))
HALO = [128, 512, 2048]
KLEN = [h + T for h in HALO]
NKEY = [h + 128 for h in HALO]
KOFF = [0, 256, 896]
NKT = 3072
SHARDED = ()
ENG = ["pe", "act", "dve", "pool", "sp"]
BLK = {"pe": "tensor", "act": "scalar", "dve": "vector", "pool": "gpsimd", "sp": "sync"}


class Prog:
    def __init__(self, nc):
        self.nc = nc
        self.ops = []
        self.semh = {}
        self.cnt = {}
        self.lastw = {}
        self.readers = {}
        self.waited = {e: {} for e in ENG}
        self.barrier = {}
        self.nid = 0
        self.bank_rr = 0
        self.nobarrier = set()

    def sem(self, name):
        if name not in self.semh:
            self.semh[name] = self.nc.alloc_semaphore("s_" + name)
        return self.semh[name]

    def bank(self):
        b = self.bank_rr
        self.bank_rr = (b + 1) % 8
        return b

    def op(self, eng, fn, r=(), w=(), slot=None, extra=(), inc=None):
        i = self.nid
        self.nid += 1
        deps = set()
        for k in list(r) + list(w):
            if k in self.lastw:
                deps.add(self.lastw[k])
        for k in w:
            deps.update(self.readers.get(k, ()))
        for k in w:
            self.lastw[k] = i
            self.readers[k] = []
        for k in r:
            self.readers.setdefault(k, []).append(i)
        self.ops.append(dict(id=i, eng=eng, fn=fn, deps=deps, slot=slot, signal=slot is not None, extra=tuple(extra), uinc=inc))
        return i

    def dma(self, q, out, in_, r=(), w=(), slot=None, extra=(), **kw):
        return self.op(q, lambda e: e.dma_start(out=out, in_=in_, **kw), r, w, slot=slot, extra=extra)

    def _wait(self, engobj, e, s, v):
        if self.waited[e].get(s, 0) >= v:
            return
        engobj.wait_ge(self.sem(s), v)
        self.waited[e][s] = v

    def flush(self):
        ops = self.ops
        self.ops = []
        byid = {o["id"]: o for o in ops}
        per = {e: [] for e in ENG}
        for o in ops:
            per[o["eng"]].append(o)
            nd = set()
            for d in o["deps"]:
                od = byid.get(d)
                if od is None:
                    continue
                if od["eng"] == "pe" and o["eng"] == "pe":
                    continue
                od["signal"] = True
                nd.add(d)
            o["deps"] = nd
        for e in ENG:
            if per[e]:
                per[e][-1]["signal"] = True
        for o in ops:
            if o["signal"]:
                s = o["slot"] or o["eng"]
                inc = o["uinc"] or (16 if o["slot"] else 1)
                self.cnt[s] = self.cnt.get(s, 0) + inc
                o["sem"], o["val"], o["inc"] = s, self.cnt[s], inc
        barrier = dict(self.barrier)
        with self.nc.Block() as block:
            for e in ENG:
                def body(engobj, e=e):
                    for s, v in barrier.items():
                        self._wait(engobj, e, s, v)
                    for o in per[e]:
                        need = {}
                        for d in o["deps"]:
                            od = byid[d]
                            need[od["sem"]] = max(need.get(od["sem"], 0), od["val"])
                        for s, v in o["extra"]:
                            need[s] = max(need.get(s, 0), v)
                        for s, v in need.items():
                            self._wait(engobj, e, s, v)
                        ins = o["fn"](engobj)
                        if o["signal"]:
                            ins.then_inc(self.sem(o["sem"]), o["inc"])
                getattr(block, BLK[e])(body)
        self.barrier = {s: v for s, v in self.cnt.items() if s not in self.nobarrier}
        self.lastw = {}
        self.readers = {}


def build(mode):
    nc = bass.Bass("TRN2", target_bir_lowering=False)
    P = Prog(nc)

    def din(name, shape, dt=F32):
        return nc.dram_tensor(name, list(shape), dt, kind="ExternalInput").ap()

    def dout(name, shape, dt=F32):
        return nc.dram_tensor(name, list(shape), dt, kind="ExternalOutput").ap()

    cf = din("cf", [128, 384])
    cb = din("cb", [128, 384], BF16)
    lnp = din("lnp", [8, 128, D])
    rw_all = din("rw", [2, D, E])
    rbb_all = din("rbb", [2, 128, E])
    bdn_all = din("bdn", [2, E, D])
    bgT_all = din("bgT", [2, 128, E * KD])
    buT_all = din("buT", [2, 128, E * KD])
    w_get = {}
    w_ag = []
    EPC = E // NCORE
    LAYERS = {"F": (0, 1), "A": (0,), "B": (1,)}[mode]
    for l_ in LAYERS:
        for t_ in "gud":
            if (l_, t_) in SHARDED:
                sh_in = din("w%s%d" % (t_, l_), [EPC * D, D])
                halves = []
                for hf in range(2):
                    nm = "%s%d%d" % (t_, l_, hf)
                    sh_loc = nc.dram_tensor("wsh_" + nm, [EPC // 2 * D, D], F32).ap()
                    full = nc.dram_tensor("wfull_" + nm, [E // 2 * D, D], F32).ap()
                    halves.append(full)
                    w_ag.append((nm, sh_in[hf * (EPC // 2) * D:(hf + 1) * (EPC // 2) * D, :], sh_loc, full))

                def get(e_, halves=halves, t_=t_, l_=l_):
                    r_, q_ = e_ // EPC, e_ % EPC
                    hf = q_ // (EPC // 2)
                    ix = r_ * (EPC // 2) + q_ % (EPC // 2)
                    return halves[hf][ix * D:(ix + 1) * D, :], (("ccw_%s%d%d" % (t_, l_, hf), 1),)
                w_get[(l_, t_)] = get
            else:
                full_in = din("w%s%d" % (t_, l_), [E, D, D])
                w_get[(l_, t_)] = (lambda e_, full_in=full_in: (full_in[e_], ()))
    x_in = din("x", [T, D])
    out_h = dout("out_h", [T, D])
    if 0 in LAYERS:
        xT_in = din("xT", [KD, 128, T + 2])
        cw_in = din("cw", [128, KD * 3])
        w_in = din("w_in", [D, 3 * D])
        w_out_a = din("w_out_a", [D, D])
        w_q = din("w_q", [D, 3 * D])
        kvw = din("kvw", [D, 3072])
    else:
        w_out_a = None
    if 1 in LAYERS:
        w_out_b = din("w_out_b", [D, D])
        ndm_in = din("ndm", [NT, 128, NKT], F16)
    else:
        w_out_b = None
    RECOMP = (mode == "F" and EXCHANGE == "recompute")
    if RECOMP:
        xh_in = [din("x_h%d" % b_, [T, D]) for b_ in (1, 2)]
        xTh_in = [din("xT_h%d" % b_, [KD, 128, T + 2]) for b_ in (1, 2)]
    if mode == "F":
        qloc = nc.dram_tensor("qloc", [48, 128, T], BF16).ap()
        kloc = nc.dram_tensor("kloc", [12 * 128, T], BF16).ap()
        vloc = nc.dram_tensor("vloc", [3 * T, 512], BF16).ap()
        if not RECOMP:
            kall = nc.dram_tensor("kall", [NCORE * 12 * 128, T], BF16).ap()
            vall = nc.dram_tensor("vall", [NCORE * 3 * T, 512], BF16).ap()
        khalo = nc.dram_tensor("khalo", [2 * 12 * 128, T], BF16).ap()
        vhalo = nc.dram_tensor("vhalo", [2 * 3 * T, 512], BF16).ap()
    elif mode == "A":
        qloc = dout("out_q", [48, 128, T], BF16)
        kloc = dout("out_k", [12 * 128, T], BF16)
        vloc = dout("out_v", [3 * T, 512], BF16)
    else:
        qloc = din("q_in", [48, 128, T], BF16)
        kloc = din("k_own", [12 * 128, T], BF16)
        vloc = din("v_own", [3 * T, 512], BF16)
        khalo = din("khalo", [2 * 12 * 128, T], BF16)
        vhalo = din("vhalo", [2 * 3 * T, 512], BF16)

    cur = [None]
    uniq = [0]

    def sb(name, shape, dt=F32):
        uniq[0] += 1
        return cur[0].enter_context(nc.sbuf_tensor("%s_%d" % (name, uniq[0]), list(shape), dt))

    root = ExitStack()
    cur[0] = root

    def phase_begin():
        st_ = ExitStack()
        cur[0] = st_
        return st_

    def phase_end(st_):
        P.flush()
        st_.close()
        cur[0] = root

    acc = sb("acc", [128, NT, D])
    h_bf = sb("h_bf", [128, NT, D], BF16)
    slab = [None, None, None]

    def alloc_slabs():
        for i in range(3):
            slab[i] = sb("slab%d" % i, [128, KD, 512], BF16)
    cf_sb = sb("cf_sb", [128, 384])
    cb_sb = sb("cb_sb", [128, 384], BF16)
    G = sb("G", [128, NT, E])
    Mf = sb("Mf", [128, NT, E])
    M_bf = sb("M_bf", [128, NT, E], BF16)
    rankm = sb("rankm", [128, NT, E])
    bg_sb = sb("bg_sb", [128, E * KD])
    bu_sb = sb("bu_sb", [128, E * KD])
    ps = [nc.alloc_psum_tensor("ps%d" % i, [128, 512], F32) for i in range(8)]
    ident_f = cf_sb[:, 0:128]
    iota_row = cf_sb[:, 128:384]
    ident_b = cb_sb[:, 0:128]
    U_b = cb_sb[:, 128:256]
    ones_b = cb_sb[:, 256:384]

    def psbf(b):
        return ps[b][:].bitcast(BF16)

    slab_rr = [0]

    def load_slab(src2d, extra=()):
        i = slab_rr[0]
        slab_rr[0] = (i + 1) % 3
        P.dma("pool", slab[i][:], src2d.rearrange("(k p) c -> p k c", p=128),
              w=[("slab", i)], slot="slab%d" % i, extra=extra)
        return i

    for (nm, sh_in, sh_loc, full) in w_ag:
        P.nobarrier.add("ccw_" + nm)
        P.nobarrier.add("wcp_" + nm)
        nq = E // NCORE // 2
        for q in range(nq):
            P.dma("sp", sh_loc[q * D:(q + 1) * D, :], sh_in[q * D:(q + 1) * D, :], w=[("wsh", nm, q)], slot="wcp_" + nm)
        P.op("pool", lambda e, sh_loc=sh_loc, full=full: e.collective_compute(
            "AllGather", ALU.bypass, replica_groups=[list(range(NCORE))], ins=[sh_loc[:, :]], outs=[full[:, :]]),
            r=[("wsh", nm, q) for q in range(nq)], w=[("wfull", nm)], slot="ccw_" + nm, inc=1)
    P.dma("sp", cf_sb[:], cf[:, :], w=["cf"], slot="cf")
    P.dma("sp", cb_sb[:], cb[:, :], w=["cb"], slot="cb")
    for tc in range(NT):
        P.dma("sp", acc[:, tc, :], (xh_in[1] if RECOMP else x_in)[tc * 128:(tc + 1) * 128, :], w=[("acc", tc)], slot="acc%d" % tc)
    P.flush()

    def out_proj(src_w, zT):
        for nb in range(4):
            si = load_slab(src_w[:, nb * 512:(nb + 1) * 512])
            for tc in range(NT):
                b = P.bank()

                def f(e, b=b, tc=tc, si=si):
                    for k in range(KD):
                        ins = e.matmul(ps[b][:], zT[:, k, tc * 128:(tc + 1) * 128], slab[si][:, k, :],
                                       start=(k == 0), stop=(k == KD - 1))
                    return ins
                P.op("pe", f, r=[("slab", si), "zT"], w=[("ps", b)])
                blk = acc[:, tc, nb * 512:(nb + 1) * 512]
                P.op("dve", lambda e, b=b, blk=blk: e.scalar_tensor_tensor(
                    out=blk, in0=blk, scalar=ALPHA, in1=ps[b][:], op0=ALU.mult, op1=ALU.add),
                    r=[("ps", b)], w=[("acc", tc)])

    PASSES = [(l_, 0) for l_ in LAYERS]
    if RECOMP:
        PASSES = [(0, 2), (0, 1), (0, 0), (1, 0)]
    first_pass = True
    for (l, hb) in PASSES:
        isA = (l == 0)
        isB = (l == 1)
        x_blk = x_in if hb == 0 else xh_in[hb - 1]
        xT_blk = (xT_in if hb == 0 else xTh_in[hb - 1]) if isA else None
        if isA and not first_pass:
            for tc in range(NT):
                P.dma("sp", acc[:, tc, :], x_blk[tc * 128:(tc + 1) * 128, :], w=[("acc", tc)], slot="acc%d" % tc)
        first_pass = False
        rw, rbb, bdn, bgT, buT = rw_all[l], rbb_all[l], bdn_all[l], bgT_all[l], buT_all[l]
        wg, wu, wd = w_get[(l, "g")], w_get[(l, "u")], w_get[(l, "d")]
        w_out = w_out_a if isA else w_out_b
        if isA:
            phs = phase_begin()
            hT = sb("xT", [128, KD, T + 2], BF16)
            alloc_slabs()
            P.dma("pool", hT[:], xT_blk.rearrange("k p t -> p k t"), w=["hT"], slot="hT")
            zT = h_bf[:].rearrange("p a (b c) -> p (a b) c", c=T)
            cw_sb = sb("cw_sb", [128, KD * 3])
            v_sb = [sb("v_sb%d" % i, [128, T + 2]) for i in range(1)]
            b_sb = [sb("b_sb%d" % i, [128, T]) for i in range(1)]
            c_sb = [sb("c_sb%d" % i, [128, 512]) for i in range(2)]
            ch_sb = sb("ch_sb", [128, 2])
            a_sb = sb("a_sb", [128, T])
            P.dma("sp", cw_sb[:], cw_in[:, :], w=["cw"], slot="cw")
            cnt = 0
            for fg in range(4):
                sb_i = load_slab(w_in[:, fg * 512:(fg + 1) * 512])
                sc_i = load_slab(w_in[:, D + fg * 512:D + (fg + 1) * 512])
                su_i = load_slab(w_in[:, 2 * D + fg * 512:2 * D + (fg + 1) * 512])
                for j in range(4):
                    fc = fg * 4 + j
                    vb = v_sb[0]
                    bb = b_sb[0]
                    js = slice(j * 128, (j + 1) * 128)
                    bh = P.bank()

                    def fh(e, bh=bh, js=js, sc_i=sc_i, su_i=su_i):
                        for k in range(KD):
                            e.matmul(ps[bh][:, 0:2], slab[sc_i][:, k, js], hT[:, k, 0:2], start=(k == 0), stop=(k == KD - 1))
                        for k in range(KD):
                            ins = e.matmul(ps[bh][:, 2:4], slab[su_i][:, k, js], hT[:, k, 0:2], start=(k == 0), stop=(k == KD - 1))
                        return ins
                    P.op("pe", fh, r=[("slab", sc_i), ("slab", su_i), "hT"], w=[("ps", bh)])
                    P.op("act", lambda e, bh=bh: e.copy(out=ch_sb[:], in_=ps[bh][:, 0:2]), r=[("ps", bh)], w=["ch"])
                    P.op("dve", lambda e, bh=bh, vb=vb: e.tensor_tensor(out=vb[:, 0:2], in0=ps[bh][:, 2:4], in1=ch_sb[:], op=ALU.mult),
                         r=[("ps", bh), "ch"], w=[("v", fc % 2)])
                    for th in range(2):
                        cols = slice(2 + th * 512, 2 + (th + 1) * 512)
                        bks = []
                        for si in (sc_i, su_i, sb_i):
                            b = P.bank()
                            bks.append(b)

                            def fm(e, b=b, si=si, js=js, cols=cols):
                                for k in range(KD):
                                    ins = e.matmul(ps[b][:], slab[si][:, k, js], hT[:, k, cols], start=(k == 0), stop=(k == KD - 1))
                                return ins
                            P.op("pe", fm, r=[("slab", si), "hT"], w=[("ps", b)])
                        cs = c_sb[cnt % 2]
                        ck = ("c", cnt % 2)
                        cnt += 1
                        P.op("act", lambda e, b=bks[0], cs=cs: e.copy(out=cs[:], in_=ps[b][:]), r=[("ps", bks[0])], w=[ck])
                        P.op("dve", lambda e, b=bks[1], cs=cs, vb=vb, cols=cols: e.tensor_tensor(
                            out=vb[:, cols], in0=ps[b][:], in1=cs[:], op=ALU.mult), r=[("ps", bks[1]), ck], w=[("v", fc % 2)])
                        P.op("act", lambda e, b=bks[2], bb=bb, th=th: e.copy(out=bb[:, th * 512:(th + 1) * 512], in_=ps[b][:]),
                             r=[("ps", bks[2])], w=[("b", fc % 2)])
                    w0 = cw_sb[:, fc * 3 + 0:fc * 3 + 1]
                    w1 = cw_sb[:, fc * 3 + 1:fc * 3 + 2]
                    w2 = cw_sb[:, fc * 3 + 2:fc * 3 + 3]
                    P.op("dve", lambda e, vb=vb, w2=w2: e.tensor_scalar(out=a_sb[:], in0=vb[:, 2:T + 2], scalar1=w2, scalar2=None, op0=ALU.mult),
                         r=[("v", fc % 2), "cw"], w=["a"])
                    P.op("dve", lambda e, vb=vb, w1=w1: e.scalar_tensor_tensor(out=a_sb[:], in0=vb[:, 1:T + 1], scalar=w1, in1=a_sb[:], op0=ALU.mult, op1=ALU.add),
                         r=[("v", fc % 2)], w=["a"])
                    P.op("dve", lambda e, vb=vb, w0=w0: e.scalar_tensor_tensor(out=a_sb[:], in0=vb[:, 0:T], scalar=w0, in1=a_sb[:], op0=ALU.mult, op1=ALU.add),
                         r=[("v", fc % 2)], w=["a"])
                    P.op("dve", lambda e, bb=bb, fc=fc: e.tensor_tensor(out=zT[:, fc, :], in0=a_sb[:], in1=bb[:], op=ALU.mult),
                         r=["a", ("b", fc % 2)], w=["zT"])
            out_proj(w_out, zT)
            phase_end(phs)

        if isB:
            phs = phase_begin()
            qt = sb("qt", [128, 3, 4, T], BF16)
            kt = [sb("kt%d" % g, [128, KLEN[g]], BF16) for g in range(3)]
            vt = [sb("vt%d" % g, [128, KLEN[g] // 128, 128], BF16) for g in range(3)]
            ndm = [sb("ndm%d" % i, [128, NKT], F16) for i in range(2)]
            sc_sb = sb("sc_sb", [128, NKT])
            p_bf2 = [sb("p_bf%d" % i, [128, NKT], BF16) for i in range(2)]
            unit_cnt = [0]
            pT2 = [sb("pT%d" % i, [128, 24, 128], BF16) for i in range(2)]
            st_mx2 = [sb("st_mx%d" % i, [128, 1]) for i in range(2)]
            st_l2 = [sb("st_l%d" % i, [128, 1]) for i in range(2)]
            st_rl2 = [sb("st_rl%d" % i, [128, 1]) for i in range(2)]
            KTK = [("kt", g_, "own") for g_ in range(3)] + [("kt", 0, "h", 0), ("kt", 1, "h", 0), ("kt", 2, "h", 0), ("kt", 2, "h", 1)]
            VTK = [("vt", g_, "own") for g_ in range(3)] + [("vt", 0, "h", 0), ("vt", 1, "h", 0), ("vt", 2, "h", 0), ("vt", 2, "h", 1)]
            pieces = []
            for g in range(3):
                o = 0
                while o < NKEY[g]:
                    n = min(512, NKEY[g] - o)
                    pieces.append((g, o, n))
                    o += n
            nd_cnt = 0
            for kvh in range(4):
                for g in range(3):
                    P.dma("sp", qt[:, g, :, :], qloc[g * 16 + kvh * 4:g * 16 + kvh * 4 + 4].rearrange("r p t -> p r t"),
                          w=["qt"], slot="qt%d" % g)
                    H = HALO[g]
                    gk = g * 4 + kvh
                    P.dma("sp", kt[g][:, H:H + T], kloc[gk * 128:(gk + 1) * 128, :], w=[("kt", g, "own")], slot="kto%d" % g)
                    P.dma("sp", vt[g][:, H // 128:H // 128 + NT, :],
                          vloc[g * T:(g + 1) * T, kvh * 128:(kvh + 1) * 128].rearrange("(c p) d -> p c d", p=128),
                          w=[("vt", g, "own")], slot="vto%d" % g)
                    for hi, back in enumerate((1, 2) if g == 2 else (1,)):
                        n = min(H, T)
                        c0 = H - back * n if g == 2 else 0
                        P.dma("sp", kt[g][:, c0:c0 + n], khalo[(back - 1) * 1536 + gk * 128:(back - 1) * 1536 + (gk + 1) * 128, T - n:T],
                              w=[("kt", g, "h", hi)], slot="kth%d_%d" % (g, hi))
                        r0 = (back - 1) * 3 * T + g * T + T - n
                        P.dma("sp", vt[g][:, c0 // 128:(c0 + n) // 128, :],
                              vhalo[r0:r0 + n, kvh * 128:(kvh + 1) * 128].rearrange("(c p) d -> p c d", p=128),
                              w=[("vt", g, "h", hi)], slot="vth%d_%d" % (g, hi))
                for bi in range(NT):
                    nd = ndm[nd_cnt % 2]
                    ndk = ("ndm", nd_cnt % 2)
                    P.dma("sp", nd[:], ndm_in[bi], w=[ndk], slot="ndm%d" % (nd_cnt % 2))
                    nd_cnt += 1
                    def attn_unit(r, kvh=kvh, bi=bi, nd=nd, ndk=ndk):
                        par = unit_cnt[0] % 2
                        unit_cnt[0] += 1
                        p_bf, pT, st_mx, st_l, st_rl = p_bf2[par], pT2[par], st_mx2[par], st_l2[par], st_rl2[par]
                        hh = kvh * 4 + r
                        slope = float(2.0 ** (-8.0 * (hh + 1) / 16.0))
                        for (g, o, n) in pieces:
                            b = P.bank()
                            P.op("pe", lambda e, b=b, g=g, o=o, n=n, r=r, bi=bi: e.matmul(
                                ps[b][:, 0:n], qt[:, g, r, bi * 128:(bi + 1) * 128], kt[g][:, bi * 128 + o:bi * 128 + o + n],
                                start=True, stop=True), r=["qt"] + KTK, w=[("ps", b)])
                            c0 = KOFF[g] + o
                            P.op("dve", lambda e, b=b, n=n, c0=c0, nd=nd, slope=slope: e.scalar_tensor_tensor(
                                out=sc_sb[:, c0:c0 + n], in0=nd[:, c0:c0 + n], scalar=slope, in1=ps[b][:, 0:n],
                                op0=ALU.mult, op1=ALU.add), r=[("ps", b), ndk], w=["sc"])
                        P.op("dve", lambda e: e.reduce_max(out=st_mx[:], in_=sc_sb[:], axis=AX.X, negate=True), r=["sc"], w=[("mx", par)])
                        P.op("act", lambda e: e.activation(out=p_bf[:], in_=sc_sb[:], func=AF.Exp, bias=st_mx[:], scale=1.0, accum_out=st_l[:]),
                             r=["sc", ("mx", par)], w=[("p", par), ("l", par)])
                        P.op("dve", lambda e: e.reciprocal(out=st_rl[:], in_=st_l[:]), r=[("l", par)], w=[("rl", par)])
                        for q3 in range(3):
                            b = P.bank()

                            def ft(e, b=b, q3=q3):
                                for i in range(8):
                                    c = q3 * 8 + i
                                    ins = e.transpose(psbf(b)[:, i * 128:(i + 1) * 128], p_bf[:, c * 128:(c + 1) * 128], ident_b)
                                return ins
                            P.op("pe", ft, r=[("p", par)], w=[("ps", b)])
                            ev = "act" if q3 < 2 else "dve"
                            if ev == "act":
                                P.op("act", lambda e, b=b, q3=q3: e.copy(out=pT[:, q3 * 8:(q3 + 1) * 8, :], in_=psbf(b).rearrange("p (a c) -> p a c", c=128)),
                                     r=[("ps", b)], w=[("pT", par, q3)])
                            else:
                                P.op("dve", lambda e, b=b, q3=q3: e.tensor_copy(out=pT[:, q3 * 8:(q3 + 1) * 8, :], in_=psbf(b).rearrange("p (a c) -> p a c", c=128)),
                                     r=[("ps", b)], w=[("pT", par, q3)])
                        b = P.bank()

                        def fpv(e, b=b, bi=bi):
                            c = 0
                            for g in range(3):
                                for ci in range(NKEY[g] // 128):
                                    ins = e.matmul(ps[b][:, 0:128], pT[:, c, :], vt[g][:, bi + ci, :], start=(c == 0), stop=(c == 23))
                                    c += 1
                            return ins
                        P.op("pe", fpv, r=[("pT", par, 0), ("pT", par, 1), ("pT", par, 2)] + VTK, w=[("ps", b)])
                        P.op("act", lambda e, b=b, bi=bi, hh=hh: e.activation(out=h_bf[:, bi, hh * 128:(hh + 1) * 128], in_=ps[b][:, 0:128],
                                                                              func=AF.Copy, scale=st_rl[:]),
                             r=[("ps", b), ("rl", par)], w=[("hbf", bi)])
                    for r in range(4):
                        attn_unit(r)
            phase_end(phs)
            phs = phase_begin()
            hT = sb("oT", [128, KD, T + 2], BF16)
            alloc_slabs()
            for tc in range(NT):
                for q2 in range(2):
                    b = P.bank()

                    def ft2(e, b=b, q2=q2, tc=tc):
                        for i in range(8):
                            k = q2 * 8 + i
                            ins = e.transpose(psbf(b)[:, i * 128:(i + 1) * 128], h_bf[:, tc, k * 128:(k + 1) * 128], ident_b)
                        return ins
                    P.op("pe", ft2, r=[("hbf", tc)], w=[("ps", b)])
                    eng = "act" if q2 == 0 else "dve"
                    if eng == "act":
                        P.op("act", lambda e, b=b, q2=q2, tc=tc: e.copy(out=hT[:, q2 * 8:(q2 + 1) * 8, tc * 128:(tc + 1) * 128],
                                                                        in_=psbf(b).rearrange("p (a c) -> p a c", c=128)), r=[("ps", b)], w=["zT"])
                    else:
                        P.op("dve", lambda e, b=b, q2=q2, tc=tc: e.tensor_copy(out=hT[:, q2 * 8:(q2 + 1) * 8, tc * 128:(tc + 1) * 128],
                                                                               in_=psbf(b).rearrange("p (a c) -> p a c", c=128)), r=[("ps", b)], w=["zT"])
            P.flush()
            out_proj(w_out, hT)
            phase_end(phs)

        phs = phase_begin()
        g_bc = sb("g_bc", [128, D])
        b_bc = sb("b_bc", [128, D])
        rw_sb = sb("rw_sb", [128, KD, E])
        rb_sb = sb("rb_sb", [128, E])
        bdn_sb = sb("bdn_sb", [E, D])
        hTt_all = sb("hTt", [128, 2, KD, 128])
        stats = sb("stats", [128, NT, 4, 6])
        mv = sb("mv", [128, NT, 2])
        std = sb("std", [128, NT, 1])
        rstd = sb("rstd", [128, NT, 1])
        lg_all = sb("lg", [128, NT, E])
        top8_all = sb("top8", [128, NT, 8])
        nmx_all = sb("nmx", [128, NT, 1])
        ex_all = sb("ex", [128, NT, E])
        ssum_all = sb("ssum", [128, NT, 1])
        rsum_all = sb("rsum", [128, NT, 1])
        GT_all = sb("GT", [E, NT, 128])
        rk_t = sb("rk_t", [128, E])

        def layer_norm(tc, which):
            stats_, mv_, std_, rstd_ = stats[:, tc], mv[:, tc], std[:, tc], rstd[:, tc]
            for i in range(4):
                P.op("dve", lambda e, i=i: e.bn_stats(out=stats_[:, i, :], in_=acc[:, tc, i * 512:(i + 1) * 512]),
                     r=[("acc", tc)], w=[("stats", tc)])
            P.op("dve", lambda e: e.bn_aggr(out=mv_[:], in_=stats_[:].rearrange("p a b -> p (a b)")), r=[("stats", tc)], w=[("mv", tc)])
            P.op("act", lambda e: e.activation(out=std_[:], in_=mv_[:, 1:2], func=AF.Sqrt, bias=EPS, scale=1.0), r=[("mv", tc)], w=[("std", tc)])
            P.op("dve", lambda e: e.reciprocal(out=rstd_[:], in_=std_[:]), r=[("std", tc)], w=[("rstd", tc)])
            P.op("dve", lambda e: e.tensor_scalar(out=acc[:, tc, :], in0=acc[:, tc, :], scalar1=mv_[:, 0:1], scalar2=rstd_[:],
                                                  op0=ALU.subtract, op1=ALU.mult), r=[("mv", tc), ("rstd", tc)], w=[("acc", tc)])
            P.op("pool", lambda e: e.tensor_tensor(out=acc[:, tc, :], in0=acc[:, tc, :], in1=g_bc[:], op=ALU.mult), r=["gbc"], w=[("acc", tc)])
            P.op("dve", lambda e: e.tensor_tensor(out=acc[:, tc, :], in0=acc[:, tc, :], in1=b_bc[:], op=ALU.add), r=["bbc"], w=[("acc", tc)])

        P.dma("sp", g_bc[:], lnp[l * 4 + 0], w=["gbc"], slot="gbc")
        P.dma("sp", b_bc[:], lnp[l * 4 + 1], w=["bbc"], slot="bbc")
        P.dma("sp", rw_sb[:], rw.rearrange("(k p) e -> p k e", p=128), w=["rw"], slot="rw")
        P.dma("sp", rb_sb[:], rbb[:, :], w=["rb"], slot="rb")
        P.dma("sp", bdn_sb[:], bdn[:, :], w=["bdn"], slot="bdn")
        P.dma("sp", bg_sb[:], bgT[:, :], w=["bg"], slot="bg")
        P.dma("sp", bu_sb[:], buT[:, :], w=["bu"], slot="bu")
        def ln1_tc(tc):
            hTt = hTt_all[:, tc % 2]
            lg, top8, nmx, ex, ssum, rsum = lg_all[:, tc], top8_all[:, tc], nmx_all[:, tc], ex_all[:, tc], ssum_all[:, tc], rsum_all[:, tc]
            GT = GT_all[:, tc]
            layer_norm(tc, 0)
            P.op("act", lambda e, tc=tc: e.copy(out=h_bf[:, tc, :], in_=acc[:, tc, :]), r=[("acc", tc)], w=[("hbf", tc)])
            bks = []
            for q4 in range(4):
                b = P.bank()
                bks.append(b)

                def ftr(e, b=b, q4=q4, tc=tc):
                    for i in range(4):
                        k = q4 * 4 + i
                        ins = e.transpose(ps[b][:, i * 128:(i + 1) * 128], acc[:, tc, k * 128:(k + 1) * 128], ident_f)
                    return ins
                P.op("pe", ftr, r=[("acc", tc)], w=[("ps", b)])
                if q4 % 2 == 0:
                    P.op("act", lambda e, b=b, q4=q4: e.copy(out=hTt[:, q4 * 4:(q4 + 1) * 4, :], in_=ps[b][:].rearrange("p (a c) -> p a c", c=128)),
                         r=[("ps", b)], w=[("hTt", tc % 2, q4)])
                else:
                    P.op("dve", lambda e, b=b, q4=q4: e.tensor_copy(out=hTt[:, q4 * 4:(q4 + 1) * 4, :], in_=ps[b][:].rearrange("p (a c) -> p a c", c=128)),
                         r=[("ps", b)], w=[("hTt", tc % 2, q4)])
            b = P.bank()

            def frt(e, b=b):
                for k in range(KD):
                    ins = e.matmul(ps[b][:, 0:E], hTt[:, k, :], rw_sb[:, k, :], start=(k == 0), stop=(k == KD - 1))
                return ins
            P.op("pe", frt, r=[("hTt", tc % 2, 0), ("hTt", tc % 2, 1), ("hTt", tc % 2, 2), ("hTt", tc % 2, 3), "rw"], w=[("ps", b)])
            P.op("dve", lambda e, b=b: e.tensor_tensor(out=lg[:], in0=ps[b][:, 0:E], in1=rb_sb[:], op=ALU.add), r=[("ps", b), "rb"], w=[("lg", tc)])
            P.op("dve", lambda e: e.max(out=top8[:], in_=lg[:]), r=[("lg", tc)], w=[("top8", tc)])
            P.op("dve", lambda e, tc=tc: e.tensor_scalar(out=Mf[:, tc, :], in0=lg[:], scalar1=top8[:, 3:4], scalar2=None, op0=ALU.is_ge),
                 r=[("lg", tc), ("top8", tc)], w=[("Mf", tc)])
            P.op("dve", lambda e: e.tensor_scalar(out=nmx[:], in0=top8[:, 0:1], scalar1=-1.0, scalar2=None, op0=ALU.mult), r=[("top8", tc)], w=[("nmx", tc)])
            P.op("act", lambda e: e.activation(out=ex[:], in_=lg[:], func=AF.Exp, bias=nmx[:], scale=1.0), r=[("lg", tc), ("nmx", tc)], w=[("ex", tc)])
            P.op("dve", lambda e, tc=tc: e.tensor_tensor(out=ex[:], in0=ex[:], in1=Mf[:, tc, :], op=ALU.mult), r=[("Mf", tc)], w=[("ex", tc)])
            P.op("dve", lambda e: e.reduce_sum(out=ssum[:], in_=ex[:], axis=AX.X), r=[("ex", tc)], w=[("ssum", tc)])
            P.op("dve", lambda e: e.reciprocal(out=rsum[:], in_=ssum[:]), r=[("ssum", tc)], w=[("rsum", tc)])
            P.op("dve", lambda e, tc=tc: e.tensor_scalar(out=G[:, tc, :], in0=ex[:], scalar1=rsum[:], scalar2=None, op0=ALU.mult),
                 r=[("ex", tc), ("rsum", tc)], w=[("G", tc)])
            P.op("dve", lambda e, tc=tc: e.tensor_copy(out=M_bf[:, tc, :], in_=Mf[:, tc, :]), r=[("Mf", tc)], w=[("Mbf", tc)])
            b = P.bank()
            P.op("pe", lambda e, b=b, tc=tc: e.transpose(ps[b][0:E, 0:128], G[:, tc, :], ident_f), r=[("G", tc)], w=[("ps", b)])
            P.op("act", lambda e, b=b: e.copy(out=GT[:], in_=ps[b][0:E, 0:128]), r=[("ps", b)], w=[("GT", tc)])
            for nb in range(4):
                b = P.bank()
                P.op("pe", lambda e, b=b, nb=nb: e.matmul(ps[b][:], GT[:], bdn_sb[:, nb * 512:(nb + 1) * 512], start=True, stop=True),
                     r=[("GT", tc), "bdn"], w=[("ps", b)])
                blk = acc[:, tc, nb * 512:(nb + 1) * 512]
                P.op("dve", lambda e, b=b, blk=blk: e.scalar_tensor_tensor(out=blk, in0=blk, scalar=ALPHA, in1=ps[b][:], op0=ALU.mult, op1=ALU.add),
                     r=[("ps", b), ("hbf", tc), ("hTt", tc % 2, 0), ("hTt", tc % 2, 1), ("hTt", tc % 2, 2), ("hTt", tc % 2, 3)], w=[("acc", tc)])
        for tc in range(NT):
            ln1_tc(tc)
        for tc in range(NT):
            b = P.bank()

            def frk(e, b=b, tc=tc):
                for t2 in range(tc):
                    e.matmul(ps[b][:, 0:E], ones_b, M_bf[:, t2, :], start=(t2 == 0), stop=False)
                return e.matmul(ps[b][:, 0:E], U_b, M_bf[:, tc, :], start=(tc == 0), stop=True)
            P.op("pe", frk, r=[("Mbf", t2) for t2 in range(tc + 1)], w=[("ps", b)])
            P.op("dve", lambda e, b=b, tc=tc: e.scalar_tensor_tensor(out=rk_t[:], in0=ps[b][:, 0:E], scalar=1.0, in1=Mf[:, tc, :],
                                                                     op0=ALU.add, op1=ALU.mult), r=[("ps", b), ("Mf", tc)], w=["rkt"])
            P.op("dve", lambda e, tc=tc: e.tensor_scalar(out=rankm[:, tc, :], in0=rk_t[:], scalar1=-1.0, scalar2=None, op0=ALU.add),
                 r=["rkt"], w=[("rankm", tc)])
        phase_end(phs)

        phs = phase_begin()
        alloc_slabs()
        xgT = sb("xgT", [128, KD, CAP], BF16)
        actT = sb("actT", [128, KD, CAP], BF16)
        SwT = sb("SwT", [128, 2, T], BF16)
        S = [sb("S%d" % i, [128, NT, CAP], BF16) for i in range(2)]
        y_sb = [sb("y_sb%d" % i, [128, 512], BF16) for i in range(2)]
        tg = [sb("tg%d" % i, [128, 2, CAP]) for i in range(2)]
        tu = [sb("tu%d" % i, [128, 2, CAP]) for i in range(2)]
        tsg = [sb("tsg%d" % i, [128, 2, CAP]) for i in range(2)]
        pc = 0
        for ex_i in range(E):
            Se = S[ex_i % 2]
            sk = ("S", ex_i % 2)
            for tc in range(NT):
                P.op("dve", lambda e, tc=tc, Se=Se, ex_i=ex_i: e.tensor_scalar(out=Se[:, tc, :], in0=iota_row, scalar1=rankm[:, tc, ex_i:ex_i + 1],
                                                                                scalar2=None, op0=ALU.is_equal), r=[], w=[sk])
            for kp in range(8):
                b = P.bank()

                def fg_(e, b=b, kp=kp, Se=Se):
                    for kk in range(2):
                        k = kp * 2 + kk
                        for tc in range(NT):
                            ins = e.matmul(ps[b][:, kk * 256:(kk + 1) * 256], h_bf[:, tc, k * 128:(k + 1) * 128], Se[:, tc, :],
                                           start=(tc == 0), stop=(tc == NT - 1))
                    return ins
                P.op("pe", fg_, r=[sk], w=[("ps", b)])
                if kp % 2 == 0:
                    P.op("act", lambda e, b=b, kp=kp: e.copy(out=xgT[:, kp * 2:kp * 2 + 2, :], in_=ps[b][:].rearrange("p (a c) -> p a c", c=CAP)),
                         r=[("ps", b)], w=["xgT"])
                else:
                    P.op("dve", lambda e, b=b, kp=kp: e.tensor_copy(out=xgT[:, kp * 2:kp * 2 + 2, :], in_=ps[b][:].rearrange("p (a c) -> p a c", c=CAP)),
                         r=[("ps", b)], w=["xgT"])
            for jc in range(2):
                b = P.bank()

                def fsw(e, b=b, jc=jc, Se=Se):
                    for tc in range(NT):
                        ins = e.transpose(psbf(b)[:, tc * 128:(tc + 1) * 128], Se[:, tc, jc * 128:(jc + 1) * 128], ident_b)
                    return ins
                P.op("pe", fsw, r=[sk], w=[("ps", b)])
                P.op("act", lambda e, b=b, jc=jc: e.copy(out=SwT[:, jc, :], in_=psbf(b)), r=[("ps", b)], w=["SwT"])
            for s in range(4):
                ig = load_slab(wg(ex_i)[0][:, s * 512:(s + 1) * 512], wg(ex_i)[1])
                iu = load_slab(wu(ex_i)[0][:, s * 512:(s + 1) * 512], wu(ex_i)[1])
                for jp in range(2):
                    bg_, bu_ = P.bank(), P.bank()
                    for (bk, si) in ((bg_, ig), (bu_, iu)):
                        def fgu(e, bk=bk, si=si, jp=jp):
                            for jj in range(2):
                                j = jp * 2 + jj
                                for k in range(KD):
                                    ins = e.matmul(ps[bk][:, jj * 256:(jj + 1) * 256], slab[si][:, k, j * 128:(j + 1) * 128], xgT[:, k, :],
                                                   start=(k == 0), stop=(k == KD - 1))
                            return ins
                        P.op("pe", fgu, r=[("slab", si), "xgT"], w=[("ps", bk)])
                    pi = pc % 2
                    pc += 1
                    fc0 = s * 4 + jp * 2
                    for jj in range(2):
                        col = ex_i * KD + fc0 + jj
                        P.op("dve", lambda e, jj=jj, col=col, pi=pi, bg_=bg_: e.tensor_scalar(
                            out=tg[pi][:, jj, :], in0=ps[bg_][:, jj * 256:(jj + 1) * 256], scalar1=bg_sb[:, col:col + 1], scalar2=7.0,
                            op0=ALU.add, op1=ALU.min), r=[("ps", bg_)], w=[("tg", pi)])
                        P.op("dve", lambda e, jj=jj, col=col, pi=pi, bu_=bu_: e.tensor_scalar(
                            out=tu[pi][:, jj, :], in0=ps[bu_][:, jj * 256:(jj + 1) * 256], scalar1=bu_sb[:, col:col + 1], scalar2=7.0,
                            op0=ALU.add, op1=ALU.min), r=[("ps", bu_)], w=[("tu", pi)])
                    P.op("act", lambda e, pi=pi: e.activation(out=tsg[pi][:], in_=tg[pi][:], func=AF.Sigmoid, scale=1.702),
                         r=[("tg", pi)], w=[("tsg", pi)])
                    P.op("dve", lambda e, pi=pi: e.tensor_scalar(out=tu[pi][:], in0=tu[pi][:], scalar1=-7.0, scalar2=1.0, op0=ALU.max, op1=ALU.add),
                         r=[], w=[("tu", pi)])
                    P.op("dve", lambda e, pi=pi: e.tensor_tensor(out=tg[pi][:], in0=tg[pi][:], in1=tsg[pi][:], op=ALU.mult),
                         r=[("tsg", pi)], w=[("tg", pi)])
                    P.op("dve", lambda e, pi=pi, fc0=fc0: e.tensor_tensor(out=actT[:, fc0:fc0 + 2, :], in0=tu[pi][:], in1=tg[pi][:], op=ALU.mult),
                         r=[("tu", pi), ("tg", pi)], w=["actT"])
            for nb in range(4):
                idn = load_slab(wd(ex_i)[0][:, nb * 512:(nb + 1) * 512], wd(ex_i)[1])
                for jc in range(2):
                    b = P.bank()

                    def fdn(e, b=b, jc=jc, idn=idn):
                        for f in range(KD):
                            ins = e.matmul(ps[b][:], actT[:, f, jc * 128:(jc + 1) * 128], slab[idn][:, f, :], start=(f == 0), stop=(f == KD - 1))
                        return ins
                    P.op("pe", fdn, r=[("slab", idn), "actT"], w=[("ps", b)])
                    P.op("act", lambda e, b=b, jc=jc: e.copy(out=y_sb[jc][:], in_=ps[b][:]), r=[("ps", b)], w=[("y", jc)])
                for tc in range(NT):
                    b = P.bank()

                    def fsc(e, b=b, tc=tc):
                        e.matmul(ps[b][:], SwT[:, 0, tc * 128:(tc + 1) * 128], y_sb[0][:], start=True, stop=False)
                        return e.matmul(ps[b][:], SwT[:, 1, tc * 128:(tc + 1) * 128], y_sb[1][:], start=False, stop=True)
                    P.op("pe", fsc, r=["SwT", ("y", 0), ("y", 1)], w=[("ps", b)])
                    blk = acc[:, tc, nb * 512:(nb + 1) * 512]
                    P.op("dve", lambda e, b=b, blk=blk, tc=tc, ex_i=ex_i: e.scalar_tensor_tensor(
                        out=blk, in0=ps[b][:], scalar=G[:, tc, ex_i:ex_i + 1], in1=blk, op0=ALU.mult, op1=ALU.add),
                        r=[("ps", b)], w=[("acc", tc)])
        phase_end(phs)

        if isA:
            st_h = ExitStack()
            cur[0] = st_h
            hT = sb("hT2", [128, KD, T + 2], BF16)
        phs = phase_begin()
        g_bc = sb("g_bc", [128, D])
        b_bc = sb("b_bc", [128, D])
        stats = sb("stats", [128, NT, 4, 6])
        mv = sb("mv", [128, NT, 2])
        std = sb("std", [128, NT, 1])
        rstd = sb("rstd", [128, NT, 1])
        P.dma("sp", g_bc[:], lnp[l * 4 + 2], w=["gbc"], slot="gbc")
        P.dma("sp", b_bc[:], lnp[l * 4 + 3], w=["bbc"], slot="bbc")
        for tc in range(NT):
            layer_norm(tc, 1)
            if isB or mode == "A":
                P.dma("sp", out_h[tc * 128:(tc + 1) * 128, :], acc[:, tc, :], r=[("acc", tc)], w=[("outh", tc)], slot="oh%d" % tc)
            if isA:
                P.op("act", lambda e, tc=tc: e.copy(out=h_bf[:, tc, :], in_=acc[:, tc, :]), r=[("acc", tc)], w=[("hbf", tc)])
                for q2 in range(2):
                    b = P.bank()

                    def ft3(e, b=b, q2=q2, tc=tc):
                        for i in range(8):
                            k = q2 * 8 + i
                            ins = e.transpose(psbf(b)[:, i * 128:(i + 1) * 128], h_bf[:, tc, k * 128:(k + 1) * 128], ident_b)
                        return ins
                    P.op("pe", ft3, r=[("hbf", tc)], w=[("ps", b)])
                    if q2 == 0:
                        P.op("act", lambda e, b=b, q2=q2, tc=tc: e.copy(out=hT[:, q2 * 8:(q2 + 1) * 8, tc * 128:(tc + 1) * 128],
                                                                        in_=psbf(b).rearrange("p (a c) -> p a c", c=128)), r=[("ps", b)], w=["hT"])
                    else:
                        P.op("dve", lambda e, b=b, q2=q2, tc=tc: e.tensor_copy(out=hT[:, q2 * 8:(q2 + 1) * 8, tc * 128:(tc + 1) * 128],
                                                                               in_=psbf(b).rearrange("p (a c) -> p a c", c=128)), r=[("ps", b)], w=["hT"])
        phase_end(phs)

        if isA:
            phs = phase_begin()
            alloc_slabs()
            store_keys = []
            qst = [sb("qst%d" % i, [128, T], BF16) for i in range(2)]
            vst = [sb("vst%d" % i, [128, 512], BF16) for i in range(2)]
            qc = 0
            for kind in (("q", "k") if hb == 0 else ("k",)):
                ncol = 12 if kind == "q" else 3
                for s in range(ncol):
                    if kind == "q":
                        si = load_slab(w_q[:, s * 512:(s + 1) * 512])
                    else:
                        si = load_slab(kvw[:, s * 512:(s + 1) * 512])
                    for j in range(4):
                        st = qst[qc % 2]
                        stk = ("qst", qc % 2)
                        for th in range(2):
                            b = P.bank()

                            def fq(e, b=b, si=si, j=j, th=th):
                                for k in range(KD):
                                    ins = e.matmul(ps[b][:], slab[si][:, k, j * 128:(j + 1) * 128], hT[:, k, th * 512:(th + 1) * 512],
                                                   start=(k == 0), stop=(k == KD - 1))
                                return ins
                            P.op("pe", fq, r=[("slab", si)], w=[("ps", b)])
                            sc_ = SCALE if kind == "q" else 1.0
                            P.op("act", lambda e, b=b, st=st, th=th, sc_=sc_: e.activation(out=st[:, th * 512:(th + 1) * 512], in_=ps[b][:],
                                                                                          func=AF.Copy, scale=sc_), r=[("ps", b)], w=[stk])
                        if kind == "q":
                            dst = qloc[s * 4 + j]
                        elif hb == 0:
                            dst = kloc[(s * 4 + j) * 128:(s * 4 + j + 1) * 128, :]
                        else:
                            dst = khalo[(hb - 1) * 1536 + (s * 4 + j) * 128:(hb - 1) * 1536 + (s * 4 + j + 1) * 128, :]
                        P.dma("sp", dst, st[:], r=[stk], w=[("oq", kind, s, j)], slot="qst%d" % (qc % 2))
                        store_keys.append(("oq", kind, s, j))
                        qc += 1
            vc = 0
            for g in range(3):
                si = load_slab(kvw[:, 1536 + g * 512:1536 + (g + 1) * 512])
                for tc in range(NT):
                    b = P.bank()

                    def fv(e, b=b, si=si, tc=tc):
                        for k in range(KD):
                            ins = e.matmul(ps[b][:], hT[:, k, tc * 128:(tc + 1) * 128], slab[si][:, k, :], start=(k == 0), stop=(k == KD - 1))
                        return ins
                    P.op("pe", fv, r=[("slab", si)], w=[("ps", b)])
                    st = vst[vc % 2]
                    stk = ("vst", vc % 2)
                    P.op("act", lambda e, b=b, st=st: e.copy(out=st[:], in_=ps[b][:]), r=[("ps", b)], w=[stk])
                    P.dma("sp", (vloc if hb == 0 else vhalo[(hb - 1) * 3 * T:hb * 3 * T, :])[g * T + tc * 128:g * T + (tc + 1) * 128, :], st[:], r=[stk], w=[("ov", g, tc)], slot="vst%d" % (vc % 2))
                    store_keys.append(("ov", g, tc))
                    vc += 1
            if mode == "F" and not RECOMP:
                P.op("pool", lambda e: e.collective_compute("AllGather", ALU.bypass, replica_groups=[list(range(NCORE))],
                                                            ins=[kloc[:, :]], outs=[kall[:, :]]),
                     r=[k_ for k_ in store_keys if k_[0] == "oq" and k_[1] == "k"], w=["kall"], slot="cck", inc=1)
                P.op("pool", lambda e: e.collective_compute("AllGather", ALU.bypass, replica_groups=[list(range(NCORE))],
                                                            ins=[vloc[:, :]], outs=[vall[:, :]]),
                     r=[k_ for k_ in store_keys if k_[0] == "ov"], w=["vall"], slot="ccv", inc=1)
                kall3 = kall.rearrange("(r x) t -> r x t", r=NCORE)
                vall3 = vall.rearrange("(r x) d -> r x d", r=NCORE)
                rk_cache = {}

                def src_rank(e, back):
                    if back not in rk_cache:
                        rk_cache[back] = e.snap((e.partition_id() + (NCORE - back)) % NCORE, min_val=0, max_val=NCORE - 1)
                    return rk_cache[back]
                for back in (1, 2):
                    P.op("sp", lambda e, back=back: e.dma_start(out=khalo[(back - 1) * 1536:back * 1536, :], in_=kall3[bass.ds(src_rank(e, back), 1)]),
                         r=["kall"], w=[("khalo", back)], slot="khalo%d" % back)
                    P.op("sp", lambda e, back=back: e.dma_start(out=vhalo[(back - 1) * 3 * T:back * 3 * T, :], in_=vall3[bass.ds(src_rank(e, back), 1)]),
                         r=["vall"], w=[("vhalo", back)], slot="vhalo%d" % back)
            phase_end(phs)
            st_h.close()
            cur[0] = root
    P.op("sp", lambda e: e.nop(), r=[], w=[])
    P.flush()
    return nc


_CACHE = {}


def _consts():
    cf = np.zeros((128, 384), np.float32)
    cf[:, 0:128] = np.eye(128, dtype=np.float32)
    cf[:, 128:384] = np.arange(256, dtype=np.float32)[None, :]
    cb = np.zeros((128, 384), np.float32)
    cb[:, 0:128] = np.eye(128)
    cb[:, 128:256] = np.triu(np.ones((128, 128)), 1)
    cb[:, 256:384] = 1.0
    return cf, cb.astype(ml_dtypes.bfloat16)


def _ndm_tables():
    out = np.full((NCORE, NT, 128, NKT), -60000.0, np.float32)
    i = np.arange(128)
    for c in range(NCORE):
        for bi in range(NT):
            tq = c * T + bi * 128 + i
            for g, (win, dil) in enumerate(GROUPS):
                tk = c * T + bi * 128 - HALO[g] + np.arange(NKEY[g])
                dist = tq[:, None] - tk[None, :]
                valid = (dist >= 0) & (dist <= win) & (dist % dil == 0) & (tk[None, :] >= 0)
                blk = np.where(valid, -dist.astype(np.float32), -60000.0)
                out[c, bi, :, KOFF[g]:KOFF[g] + NKEY[g]] = blk
    return out.astype(np.float16)


FUSED = True
EXCHANGE = "recompute"


def kernel(x, a_w_in, a_conv_w, a_w_out, b_w_q, b_w_o, kv_w,
           ln_mix_g, ln_mix_b, ln_ffn_g, ln_ffn_b, router_w, router_b,
           exp_w_gate, exp_b_gate, exp_w_up, exp_b_up, exp_w_down, exp_b_down):
    f = lambda a: np.asarray(a, np.float32)
    xs = f(x)[0]
    cf, cb = _consts()
    lnp = np.ascontiguousarray(np.stack([np.broadcast_to(f(v)[l][None, :], (128, D)) for l in range(2)
                                         for v in (ln_mix_g, ln_mix_b, ln_ffn_g, ln_ffn_b)]).astype(np.float32))
    bgT = np.ascontiguousarray(f(exp_b_gate).reshape(2, E, KD, 128).transpose(0, 3, 1, 2).reshape(2, 128, E * KD))
    buT = np.ascontiguousarray(f(exp_b_up).reshape(2, E, KD, 128).transpose(0, 3, 1, 2).reshape(2, 128, E * KD))
    rbb = np.ascontiguousarray(np.broadcast_to(f(router_b)[:, None, :], (2, 128, E)))
    cw = np.ascontiguousarray(f(a_conv_w)[0].reshape(3, KD, 128).transpose(2, 1, 0).reshape(128, KD * 3))
    xpad = np.concatenate([np.zeros((2, D), np.float32), xs], axis=0)
    ndm = _ndm_tables()
    com = dict(cf=cf, cb=cb, lnp=lnp, rw=f(router_w), rbb=rbb, bdn=f(exp_b_down), bgT=bgT, buT=buT)
    lay = {0: dict(cw=cw, w_in=f(a_w_in)[0], w_out_a=f(a_w_out)[0], w_q=f(b_w_q)[0], kvw=f(kv_w)),
           1: dict(w_out_b=f(b_w_o)[0])}

    def weights(m, c, layers):
        for l_ in layers:
            for t_, arr in (("g", exp_w_gate), ("u", exp_w_up), ("d", exp_w_down)):
                a = f(arr)[l_]
                if (l_, t_) in SHARDED:
                    m["w%s%d" % (t_, l_)] = a[c * (E // NCORE):(c + 1) * (E // NCORE)].reshape(E // NCORE * D, D)
                else:
                    m["w%s%d" % (t_, l_)] = a

    def run(mode, per_core):
        if mode not in _CACHE:
            _CACHE[mode] = build(mode)
        layers = {"F": (0, 1), "A": (0,), "B": (1,)}[mode]
        in_maps = []
        for c in range(NCORE):
            m = dict(com)
            for l_ in layers:
                m.update(lay[l_])
            weights(m, c, layers)
            m.update(per_core(c))
            in_maps.append(m)
        return run_bass_kernel_spmd(_CACHE[mode], in_maps, core_ids=list(range(NCORE))).results

    def l0_inputs(c):
        return dict(x=np.ascontiguousarray(xs[c * T:(c + 1) * T]),
                    xT=np.ascontiguousarray(xpad[c * T:c * T + T + 2].T.reshape(KD, 128, T + 2)))

    def halo_inputs(c):
        d = {}
        if EXCHANGE == "recompute":
            xz = np.concatenate([np.zeros((2 * T + 2, D), np.float32), xs], axis=0)
            for b_ in (1, 2):
                t0 = (c - b_) * T
                d["x_h%d" % b_] = np.ascontiguousarray(xz[t0 + 2 * T + 2:t0 + 3 * T + 2])
                d["xT_h%d" % b_] = np.ascontiguousarray(xz[t0 + 2 * T:t0 + 3 * T + 2].T.reshape(KD, 128, T + 2))
        return d

    if FUSED:
        res = run("F", lambda c: dict(l0_inputs(c), ndm=ndm[c], **halo_inputs(c)))
    else:
        ra = run("A", l0_inputs)
        res = run("B", lambda c: dict(
            x=ra[c]["out_h"], q_in=ra[c]["out_q"], k_own=ra[c]["out_k"], v_own=ra[c]["out_v"], ndm=ndm[c],
            khalo=np.concatenate([ra[(c - 1) % NCORE]["out_k"], ra[(c - 2) % NCORE]["out_k"]], axis=0),
            vhalo=np.concatenate([ra[(c - 1) % NCORE]["out_v"], ra[(c - 2) % NCORE]["out_v"]], axis=0)))
    out = np.concatenate([r["out_h"] for r in res], axis=0)
    return out.reshape(1, NCORE * T, D).astype(np.float32)
```
